# Optimizing a Trainium2 kernel written in Bass

```python
import math
import jax, jax.numpy as jnp
from jax import lax
import numpy as np

D_MODEL = 4096
BATCH = 8
SEQ = 2048
DEPTH = 2

D_MIX = D_MODEL
D_ATTN = D_MIX // 2
D_POOL = D_MIX - D_ATTN
HEAD_DIM = 64
N_Q_HEADS = D_ATTN // HEAD_DIM
N_KV_HEADS = max(1, N_Q_HEADS // 8)
GQA_GROUP = N_Q_HEADS // N_KV_HEADS
D_KV = N_KV_HEADS * HEAD_DIM
WINDOW = 128
BLOCK = 128
POOL_WINDOWS = (2, 4, 8, 16)
N_POOL_GROUPS = len(POOL_WINDOWS)
POOL_GROUP_DIM = D_POOL // N_POOL_GROUPS
D_IN = D_ATTN + 2 * D_KV + D_POOL
D_FF = ((8 * D_MODEL // 3 + 255) // 256) * 256
N_EXPERTS = 8
TOP_K = 2
MOE_D_FF = D_MODEL
N_DENSE = (DEPTH + 1) // 2
N_MOE = DEPTH // 2
N_MOD = 6
EPS = 1e-6
NEG_INF = -1e30

kernel_name = 'hybrid_swa_pool_moe_block'


def _rmsnorm(x, g):
    xf = x.astype(jnp.float32)
    y = xf * lax.rsqrt(jnp.mean(xf * xf, axis=-1, keepdims=True) + EPS)
    return (y * g.astype(jnp.float32)).astype(x.dtype)


def _modulate(h, shift, scale):
    return h * (1 + scale[:, None, :]) + shift[:, None, :]


def _alibi_slopes():
    h = jnp.arange(1, N_Q_HEADS + 1, dtype=jnp.float32)
    return jnp.exp2(-8.0 * h / N_Q_HEADS)


def _sliding_window_attention(q, k, v, sinks):
    B, S, _ = q.shape
    nb = S // BLOCK
    q = q.reshape(B, nb, BLOCK, N_KV_HEADS, GQA_GROUP, HEAD_DIM)
    k = k.reshape(B, S, N_KV_HEADS, HEAD_DIM)
    v = v.reshape(B, S, N_KV_HEADS, HEAD_DIM)
    pad = ((0, 0), (BLOCK, 0), (0, 0), (0, 0))
    kb = jnp.pad(k, pad).reshape(B, nb + 1, BLOCK, N_KV_HEADS, HEAD_DIM)
    vb = jnp.pad(v, pad).reshape(B, nb + 1, BLOCK, N_KV_HEADS, HEAD_DIM)
    k_win = jnp.concatenate([kb[:, :-1], kb[:, 1:]], axis=2)
    v_win = jnp.concatenate([vb[:, :-1], vb[:, 1:]], axis=2)
    scores = jnp.einsum('bnikgd,bnjkd->bnkgij', q, k_win).astype(jnp.float32)
    scores = scores * (HEAD_DIM ** -0.5)
    i = jnp.arange(BLOCK)[:, None]
    j = jnp.arange(2 * BLOCK)[None, :]
    dist = i + BLOCK - j
    s_abs = jnp.arange(nb)[:, None, None] * BLOCK - BLOCK + j[None]
    valid = (dist[None] >= 0) & (dist[None] < WINDOW) & (s_abs >= 0)
    slopes = _alibi_slopes().reshape(N_KV_HEADS, GQA_GROUP)
    alibi = -slopes[:, :, None, None] * dist.astype(jnp.float32)
    scores = jnp.where(valid[None, :, None, None], scores + alibi[None, None], NEG_INF)
    sink = jnp.broadcast_to(sinks.astype(jnp.float32).reshape(N_KV_HEADS, GQA_GROUP, 1, 1),
                            scores.shape[:-1] + (1,))
    probs = jax.nn.softmax(jnp.concatenate([scores, sink], axis=-1), axis=-1)[..., :-1]
    out = jnp.einsum('bnkgij,bnjkd->bnikgd', probs.astype(v.dtype), v_win)
    return out.reshape(B, S, D_ATTN)


def _multiscale_pool(u, w_pool, pool_scale):
    B, S, _ = u.shape
    ug = u.reshape(B, S, N_POOL_GROUPS, POOL_GROUP_DIM).astype(jnp.float32)
    cs = jnp.pad(jnp.cumsum(ug, axis=1), ((0, 0), (1, 0), (0, 0), (0, 0)))
    t = jnp.arange(S)[:, None]
    win = jnp.array(POOL_WINDOWS, dtype=jnp.int32)[None, :]
    lo = jnp.maximum(t + 1 - win, 0)
    count = (t + 1 - lo).astype(jnp.float32)
    g_idx = jnp.arange(N_POOL_GROUPS)[None, :]
    window_sum = cs[:, 1:] - cs[:, lo, g_idx]
    pooled = window_sum / count[None, :, :, None] - ug
    y = jnp.einsum('bsgc,gcd->bsgd', pooled.astype(u.dtype), w_pool)
    return y.reshape(B, S, D_POOL) * pool_scale


def _swiglu(h, w_gate, w_up, w_down):
    return (jax.nn.silu(h @ w_gate) * (h @ w_up)) @ w_down


def _moe_swiglu(h, router_w, router_b, w_gate, w_up, w_down):
    B, S, D = h.shape
    hf = h.reshape(B * S, D)
    logits = (hf @ router_w).astype(jnp.float32) + router_b.astype(jnp.float32)
    top_vals, top_idx = lax.top_k(logits, TOP_K)
    top_w = jax.nn.softmax(top_vals, axis=-1)
    combine = jnp.sum(jax.nn.one_hot(top_idx, N_EXPERTS, dtype=jnp.float32) * top_w[..., None],
                      axis=1).astype(h.dtype)
    out = jnp.zeros_like(hf)
    for e in range(N_EXPERTS):
        out = out + combine[:, e:e + 1] * _swiglu(hf, w_gate[e], w_up[e], w_down[e])
    return out.reshape(B, S, D)


def setup_inputs(seed: int = 0) -> dict:
    key = jax.random.key(seed)
    ks = jax.random.split(key, 24)
    f32 = jnp.float32
    nrm = lambda k, shape, s: jax.random.normal(k, shape, f32) * s
    gain = lambda k, shape: 1.0 + 0.05 * jax.random.normal(k, shape, f32)
    return {
        'x': nrm(ks[0], (BATCH, SEQ, D_MODEL), 1.0),
        'c': nrm(ks[1], (BATCH, D_MODEL), 1.0),
        'w_ada': nrm(ks[2], (DEPTH, D_MODEL, N_MOD * D_MODEL), 0.5 * D_MODEL ** -0.5),
        'b_ada': nrm(ks[3], (DEPTH, N_MOD * D_MODEL), 0.01),
        'norm_pre_mix': gain(ks[4], (DEPTH, D_MODEL)),
        'norm_post_mix': gain(ks[5], (DEPTH, D_MODEL)),
        'norm_pre_ffn': gain(ks[6], (DEPTH, D_MODEL)),
        'norm_post_ffn': gain(ks[7], (DEPTH, D_MODEL)),
        'w_in': nrm(ks[8], (DEPTH, D_MODEL, D_IN), D_MODEL ** -0.5),
        'sinks': nrm(ks[9], (DEPTH, N_Q_HEADS), 1.0),
        'w_pool': nrm(ks[10], (DEPTH, N_POOL_GROUPS, POOL_GROUP_DIM, POOL_GROUP_DIM), POOL_GROUP_DIM ** -0.5),
        'pool_scale': gain(ks[11], (DEPTH, D_POOL)),
        'w_out': nrm(ks[12], (DEPTH, D_MIX, D_MODEL), D_MIX ** -0.5),
        'ffn_w_gate': nrm(ks[13], (N_DENSE, D_MODEL, D_FF), D_MODEL ** -0.5),
        'ffn_w_up': nrm(ks[14], (N_DENSE, D_MODEL, D_FF), D_MODEL ** -0.5),
        'ffn_w_down': nrm(ks[15], (N_DENSE, D_FF, D_MODEL), D_FF ** -0.5),
        'router_w': nrm(ks[16], (N_MOE, D_MODEL, N_EXPERTS), D_MODEL ** -0.5),
        'router_b': nrm(ks[17], (N_MOE, N_EXPERTS), 0.01),
        'moe_w_gate': nrm(ks[18], (N_MOE, N_EXPERTS, D_MODEL, MOE_D_FF), D_MODEL ** -0.5),
        'moe_w_up': nrm(ks[19], (N_MOE, N_EXPERTS, D_MODEL, MOE_D_FF), D_MODEL ** -0.5),
        'moe_w_down': nrm(ks[20], (N_MOE, N_EXPERTS, MOE_D_FF, D_MODEL), MOE_D_FF ** -0.5),
    }


def reference(x, c, w_ada, b_ada, norm_pre_mix, norm_post_mix, norm_pre_ffn, norm_post_ffn,
              w_in, sinks, w_pool, pool_scale, w_out, ffn_w_gate, ffn_w_up, ffn_w_down,
              router_w, router_b, moe_w_gate, moe_w_up, moe_w_down):
    c_act = jax.nn.silu(c)
    for l in range(DEPTH):
        mod = c_act @ w_ada[l] + b_ada[l]
        shift1, scale1, gate1, shift2, scale2, gate2 = jnp.split(mod, N_MOD, axis=-1)
        h = _modulate(_rmsnorm(x, norm_pre_mix[l]), shift1, scale1)
        proj = h @ w_in[l]
        q = proj[..., :D_ATTN]
        k = proj[..., D_ATTN:D_ATTN + D_KV]
        v = proj[..., D_ATTN + D_KV:D_ATTN + 2 * D_KV]
        u = proj[..., D_ATTN + 2 * D_KV:]
        attn = _sliding_window_attention(q, k, v, sinks[l])
        pool = _multiscale_pool(u, w_pool[l], pool_scale[l])
        mix = jnp.concatenate([attn, pool], axis=-1) @ w_out[l]
        x = x + gate1[:, None, :] * _rmsnorm(mix, norm_post_mix[l])
        h2 = _modulate(_rmsnorm(x, norm_pre_ffn[l]), shift2, scale2)
        if l % 2 == 0:
            d = l // 2
            f = _swiglu(h2, ffn_w_gate[d], ffn_w_up[d], ffn_w_down[d])
        else:
            m = l // 2
            f = _moe_swiglu(h2, router_w[m], router_b[m], moe_w_gate[m], moe_w_up[m], moe_w_down[m])
        x = x + gate2[:, None, :] * _rmsnorm(f, norm_post_ffn[l])
    return x
```

```python
import numpy as np
import ml_dtypes
from contextlib import ExitStack
import concourse.bass as bass
import concourse.mybir as mybir
from concourse.bass_utils import run_bass_kernel_spmd

F32 = mybir.dt.float32
BF16 = mybir.dt.bfloat16
AF = mybir.ActivationFunctionType
ALU = mybir.AluOpType
AX = mybir.AxisListType

NCORES = 8
D = 4096
S = 2048
KC = D // 128
T = 512
NT = S // T
NB = T // 128
D_IN = 4608
D_FF = 11008
NE = 8
NS = 3
BIG = 1.0e30
EPS = 1e-6
C_DM, C_DM0, C_NSL, C_INV, C_EPS, CW = 0, 256, 512, 544, 560, 576


class Prog:
    def __init__(self):
        self.reset()

    def reset(self):
        self.streams = {e: [] for e in ("pe", "act", "dve", "pool", "sp")}
        self.cnt = {}
        self.waited = {e: {} for e in self.streams}
        self.bufs = {}
        self.overlaps = {}
        self.dry = False

    def op(self, eng, fn, reads=(), writes=(), dma=None):
        if self.dry:
            return None
        banks = tuple(dict.fromkeys(b for b in list(reads) + list(writes) if b.startswith("ps") and b[2].isdigit()))
        deps = []
        xdeps = []
        for b in banks:
            st = self.bufs.get("B" + b)
            if st:
                if st[0]:
                    xdeps.append(st[0])
                xdeps.extend(st[1].items())
        for b in reads:
            for bb in [b] + self.overlaps.get(b, []):
                st = self.bufs.get(bb)
                if st and st[0]:
                    deps.append(st[0])
        for b in writes:
            for bb in [b] + self.overlaps.get(b, []):
                st = self.bufs.get(bb)
                if st:
                    if st[0]:
                        deps.append(st[0])
                    deps.extend(st[1].items())
        own = "c:" + eng
        waits = {}
        wd = self.waited[eng]
        for key, val in deps:
            if key == own and eng == "pe":
                continue
            if wd.get(key, 0) >= val:
                continue
            if waits.get(key, 0) < val:
                waits[key] = val
        for key, val in xdeps:
            if key == own:
                continue
            if wd.get(key, 0) >= val:
                continue
            if waits.get(key, 0) < val:
                waits[key] = val
        for k, v in waits.items():
            wd[k] = v
        key = ("d:" + dma) if dma else own
        inc = 16 if dma else 1
        self.cnt[key] = self.cnt.get(key, 0) + inc
        tok = (key, self.cnt[key])
        self.streams[eng].append((list(waits.items()), fn, key, inc))
        for b in writes:
            self.bufs[b] = [tok, {}]
        for b in banks:
            self.bufs["B" + b] = [tok, {}]
        for b in reads:
            st = self.bufs.setdefault(b, [None, {}])
            if st[1].get(key, 0) < tok[1]:
                st[1][key] = tok[1]
        return tok


class Rot:
    def __init__(self, items):
        self.items = items
        self.i = 0

    def next(self):
        it = self.items[self.i % len(self.items)]
        self.i += 1
        return it


def build_program(stage=None):
    dbgmode = stage is not None
    nc = bass.Bass("TRN2", target_bir_lowering=False)
    P = Prog()

    def din(name, shape, dt=F32):
        return nc.dram_tensor(name, list(shape), dt, kind="ExternalInput").ap()

    x_d = din("x", [S, D])
    cT_d = din("cT", [128, KC])
    w_ada_d = din("w_ada", [2, D, 6 * D])
    badaT_d = din("b_adaT", [128, 384])
    gains_d = din("gainsT", [128, 256])
    w_in_d = din("w_in", [2, D, D_IN])
    sinks_d = din("sinks_b", [128, 64])
    w_pool_d = din("w_pool", [2, 4, 512, 512])
    pscale_d = din("pool_scaleT", [128, 32])
    w_out_d = din("w_out", [2, D, D])
    small = dbgmode and stage.get("ffn_tiles", NT) == 0 and stage.get("layers", 2) == 1
    if small:
        fg_d = fu_d = fd_d = mg_d = mu_d = md_d = None
    else:
        fg_d = din("ffn_w_gate", [1, D, D_FF])
        fu_d = din("ffn_w_up", [1, D, D_FF])
        fd_d = din("ffn_w_down", [1, D_FF, D])
    rw_d = din("router_w", [1, D, NE])
    rb_d = din("router_b_b", [128, NE])
    if not small:
        mg_d = din("moe_w_gate", [1, NE, D, D])
        mu_d = din("moe_w_up", [1, NE, D, D])
        md_d = din("moe_w_down", [1, NE, D, D])
    ident_d = din("ident", [128, 128], BF16)
    cst_d = din("cst", [128, CW])
    out_d = nc.dram_tensor("out", [S, D], F32, kind="ExternalOutput").ap()
    if dbgmode:
        xs1_d = nc.dram_tensor("xs1", [S, D], F32, kind="ExternalOutput").ap()
        xs2_d = nc.dram_tensor("xs2", [S, D], F32, kind="ExternalOutput").ap()
        dbg_d = nc.dram_tensor("dbg", [128, 4096], F32, kind="ExternalOutput").ap()
    else:
        xs1_d = nc.dram_tensor("xs1", [S, D], F32).ap()
        xs2_d = nc.dram_tensor("xs2", [S, D], F32).ap()
    gps_d = nc.dram_tensor("gps", [4, D], F32).ap()
    fin_d = nc.dram_tensor("fin", [1, 64], F32).ap()

    def sb(name, shape, dt):
        return nc.alloc_sbuf_tensor(name, list(shape), dt)

    cst = sb("cst_s", [128, CW], F32)
    ident = sb("ident_s", [128, 128], BF16)
    cTs = sb("cT_s", [128, KC], F32)
    cact = sb("cact", [128, KC], BF16)
    badaT = sb("badaT_s", [128, 384], F32)
    gainsT = sb("gains_s", [128, 256], F32)
    sinks = sb("sinks_s", [128, 64], F32)
    pscale = sb("pscale_s", [128, 32], F32)
    rb = sb("rb_s", [128, NE], F32)
    rw = sb("rw_s", [128, KC * NE], BF16)
    modc = sb("modc", [128, 384], F32)
    gs = sb("gs", [128, 4 * KC], F32)
    gpc = sb("gpc", [128, 4 * KC], F32)
    wbuf = [sb(f"wb{i}", [128, 8192], BF16) for i in range(NS)]
    hT = sb("hT", [128, KC * T], BF16)
    xin = [sb(f"xin{h}", [128, 2048], F32) for h in range(2)]
    x8 = sb("x8", [128, D], BF16)
    stat = sb("stat", [128, 32], F32)
    ssp = sb("ssp", [128, 64], F32)
    ast = sb("ast", [128, 16 * 8], F32)
    rstat = sb("rstat", [128, 64], F32)
    comb = sb("comb", [128, NB * NE], F32)
    utail = sb("utail", [128, 256], F32)
    junk = sb("junk", [128, 1024], BF16)
    UW = 43008
    U = sb("U", [128, UW], BF16)

    xn = x8[:, :]
    gph = x8[:, :].bitcast(F32)
    hT3 = hT[:, :].rearrange("p (k t) -> p k t", t=T)
    rw3 = rw[:, :].rearrange("p (k e) -> p k e", e=NE)

    def ub(o, n):
        return U[:, o:o + n]

    def uf(o, n):
        return U[:, o:o + 2 * n].bitcast(F32)

    catT3 = ub(0, 16384).rearrange("p (c t) -> p c t", t=T)
    kT3 = ub(16384, 5120).rearrange("p (h t) -> p h t", t=640)
    vpad4 = ub(21504, 3840).rearrange("p (s h d) -> p s h d", h=4, d=192)
    ZA = 25344
    mixb3 = ub(ZA, 16384).rearrange("p (b n) -> p b n", n=D)
    o = ZA
    qT = [ub(o, 512), ub(o + 512, 512)]; o += 1024
    ucur = uf(o, 528); o += 1056
    stt_ = [uf(o, 528), uf(o + 1056, 528)]; o += 2112
    fixb = uf(o, 16); o += 32
    pooled3 = ub(o, 2048).rearrange("p (i t) -> p i t", t=T); o += 2048
    ssb = [uf(o, 256), uf(o + 512, 256)]; o += 1024
    pexp = [ub(o, 256), ub(o + 256, 256)]; o += 512
    pnb = [ub(o + i * 256, 256) for i in range(16)]; o += 4096
    pts = [ub(o + i * 1024, 1024) for i in range(2)]; o += 2048
    assert o <= ZA + 16384
    acc3 = uf(0, 16384).rearrange("p (b n) -> p b n", n=D)
    hid3 = ub(32768, 8192).rearrange("p (j t) -> p j t", t=T)
    sg = [uf(40960, 512), uf(41984, 512)]

    HT_ALL = tuple(f"hT{b}_{g}" for b in range(NB) for g in range(4))
    CAT_ALL = tuple(f"catT{c}" for c in range(32))
    KT_ALL = tuple(f"kT{k}" for k in range(4))
    VP_ALL = tuple(f"vpad{k}" for k in range(5))
    MIXB_ALL = tuple(f"mixb{b}" for b in range(NB))
    ACC_ALL = tuple(f"acc{b}" for b in range(NB))
    HID_ALL = tuple(f"hid{j}" for j in range(16))
    PO_ALL = tuple(f"pooled{i}" for i in range(4))
    base_names = list(CAT_ALL) + list(KT_ALL) + list(VP_ALL)
    zone_names = (["qT0", "qT1", "ucur", "st0", "st1", "fixb"] + list(PO_ALL) + ["ssb0", "ssb1", "pexp0", "pexp1"]
                  + [f"pn{i}" for i in range(16)] + [f"pts{i}" for i in range(2)])
    mixer_names = base_names + list(MIXB_ALL) + zone_names
    ffn_names = list(ACC_ALL) + list(HID_ALL) + ["sg0", "sg1"]
    for n in ffn_names:
        P.overlaps[n] = list(mixer_names)
    for n in mixer_names:
        P.overlaps[n] = list(ffn_names)
    for n in MIXB_ALL:
        P.overlaps[n] = P.overlaps[n] + zone_names
    for n in zone_names:
        P.overlaps[n] = P.overlaps[n] + list(MIXB_ALL)
    overlaps0 = {k: list(v) for k, v in P.overlaps.items()}

    psb = [nc.alloc_psum_tensor(f"ps{i}", [128, 512], F32) for i in range(8)]

    def psf(i):
        return psb[i][:, :]

    def psh(i):
        return psb[i][:, :].bitcast(BF16)

    def slot_view(s, shape):
        a, b = shape
        return wbuf[s][:, 0:a * b].rearrange("p (a b) -> p a b", b=b)

    class WStream:
        def __init__(self):
            self.plan = []
            self.pos = 0
            self.loaded = 0

        def get(self, src, shape):
            if P.dry:
                self.plan.append((src, shape))
                return "wb0", slot_view(0, shape)
            i = self.pos
            self.pos += 1
            while self.loaded < min(i + NS, len(self.plan)):
                j = self.loaded
                s = j % NS
                srcj, shp = self.plan[j]
                view = slot_view(s, shp)
                P.op("pool", (lambda e, view=view, srcj=srcj: e.dma_start(out=view, in_=srcj)),
                     reads=(), writes=(f"wb{s}",), dma=f"wb{s}")
                self.loaded += 1
            return f"wb{i % NS}", slot_view(i % NS, self.plan[i][1])

    ws = WStream()

    def kview(ap2d):
        return ap2d.rearrange("(k p) n -> p k n", p=128)

    def mm_group(ps_ap, pairs):
        def fn(e):
            n = len(pairs)
            ins = None
            for i, (l_, r_) in enumerate(pairs):
                ins = e.matmul(ps_ap, l_, r_, start=(i == 0), stop=(i == n - 1))
            return ins
        return fn

    def emit_setup():
        loads = [(cst, cst_d), (ident, ident_d), (cTs, cT_d), (badaT, badaT_d), (gainsT, gains_d),
                 (sinks, sinks_d), (pscale, pscale_d), (rb, rb_d)]
        for t_, d_ in loads:
            P.op("sp", (lambda e, t_=t_, d_=d_: e.dma_start(out=t_[:, :], in_=d_[:, :])),
                 reads=(), writes=("setup",), dma="setup")
        P.op("pool", lambda e: e.dma_start(out=rw3, in_=rw_d[0].rearrange("(k p) e -> p k e", p=128)),
             reads=(), writes=("rw",), dma="rw")
        P.op("act", lambda e: e.activation(out=cact[:, :], in_=cTs[:, :], func=AF.Silu),
             reads=("setup",), writes=("cact",))
        P.op("dve", lambda e: e.memset(U[:, 21504:21504 + 3840], 0.0), reads=(), writes=VP_ALL)
        for l in range(2):
            pname = f"ps{l}"
            mps = psf(l)
            for t in range(96):
                nm, wv = ws.get(kview(w_ada_d[l, :, t * 256:(t + 1) * 256]), (KC, 256))

                def fn(e, wv=wv, t=t, mps=mps):
                    ins = None
                    for j in range(2):
                        jc = t * 2 + j
                        for kc in range(KC):
                            ins = e.matmul(mps[:, jc:jc + 1], wv[:, kc, j * 128:(j + 1) * 128],
                                           cact[:, kc:kc + 1], start=(kc == 0), stop=(kc == KC - 1))
                    return ins
                P.op("pe", fn, reads=(nm, "cact"), writes=(pname,))
            P.op("dve", (lambda e, l=l, mps=mps: e.tensor_tensor(
                out=modc[:, l * 192:(l + 1) * 192], in0=mps[:, 0:192], in1=badaT[:, l * 192:(l + 1) * 192],
                op=ALU.add)), reads=(pname, "setup"), writes=("modc",))
            for sub in range(2):
                gi = l * 2 + sub

                def fn(e, l=l, sub=sub, gi=gi):
                    sc = modc[:, l * 192 + (1 + 3 * sub) * 32: l * 192 + (2 + 3 * sub) * 32]
                    gpre = gainsT[:, (2 * sub) * 64 + l * 32:(2 * sub) * 64 + l * 32 + 32]
                    e.scalar_tensor_tensor(out=gs[:, gi * 32:(gi + 1) * 32], in0=sc, scalar=1.0, in1=gpre,
                                           op0=ALU.add, op1=ALU.mult)
                    gt = modc[:, l * 192 + (2 + 3 * sub) * 32: l * 192 + (3 + 3 * sub) * 32]
                    gpost = gainsT[:, (2 * sub + 1) * 64 + l * 32:(2 * sub + 1) * 64 + l * 32 + 32]
                    return e.tensor_tensor(out=gpc[:, gi * 32:(gi + 1) * 32], in0=gt, in1=gpost, op=ALU.mult)
                P.op("dve", fn, reads=("modc", "setup"), writes=("gs", "gpc"))

                def fn2(e, gi=gi):
                    with nc.allow_non_contiguous_dma(reason="tiny column->row relayout"):
                        return e.dma_start(out=gps_d[gi].rearrange("(k p) -> p k", p=128),
                                           in_=gpc[:, gi * 32:(gi + 1) * 32])
                P.op("sp", fn2, reads=("gpc",), writes=("gps",), dma="gps")

    tp_rot = None
    mm_rot = None

    def phase_A(tt, xsrc, xname, l, sub):
        gscol = (l * 2 + sub) * 32
        shcol = l * 192 + 3 * sub * 32
        for blk in range(NB):
            gb = tt * NB + blk
            r0 = gb * 128
            sc = (gb % 2) * 8
            stn = f"stat{gb % 2}"
            P.op("dve", (lambda e, sc=sc: e.memset(stat[:, sc:sc + 2], 0.0)), reads=(), writes=(stn,))
            for h in range(2):
                P.op("sp", (lambda e, h=h, r0=r0: e.dma_start(out=xin[h][:, :],
                                                               in_=xsrc[r0:r0 + 128, h * 2048:(h + 1) * 2048])),
                     reads=(f"{xname}:{gb}:{h}",), writes=(f"xin{h}",), dma=f"xin{h}")
                P.op("act", (lambda e, h=h, sc=sc: e.activation(out=xn[:, h * 2048:(h + 1) * 2048], in_=xin[h][:, :],
                                                                func=AF.Square, accum_out=stat[:, sc + h:sc + h + 1])),
                     reads=(f"xin{h}",), writes=(f"x8_{h}", stn))
            P.op("dve", (lambda e, sc=sc: e.tensor_tensor(out=stat[:, sc + 2:sc + 3], in0=stat[:, sc:sc + 1],
                                                          in1=stat[:, sc + 1:sc + 2], op=ALU.add)),
                 reads=(stn,), writes=(stn,))
            P.op("act", (lambda e, sc=sc: e.activation(out=stat[:, sc + 3:sc + 4], in_=stat[:, sc + 2:sc + 3],
                                                       func=AF.Sqrt, bias=cst[:, C_EPS:C_EPS + 1], scale=1.0 / D)),
                 reads=(stn, "setup"), writes=(stn,))
            P.op("dve", (lambda e, sc=sc: e.reciprocal(out=stat[:, sc + 4:sc + 5], in_=stat[:, sc + 3:sc + 4])),
                 reads=(stn,), writes=(stn,))
            for h in range(2):
                P.op("dve", (lambda e, h=h, sc=sc: e.tensor_scalar(
                    out=xn[:, h * 2048:(h + 1) * 2048], in0=xin[h][:, :], scalar1=stat[:, sc + 4:sc + 5],
                    scalar2=None, op0=ALU.mult)), reads=(f"xin{h}", stn), writes=(f"x8_{h}",))
            for g in range(4):
                tpn, tpi = tp_rot.next()
                tpv = psh(tpi)

                def fn(e, g=g, tpv=tpv):
                    ins = None
                    for j in range(8):
                        kc = g * 8 + j
                        ins = e.transpose(out=tpv[:, j * 128:(j + 1) * 128], in_=xn[:, kc * 128:(kc + 1) * 128],
                                          identity=ident[:, :])
                    return ins
                P.op("pe", fn, reads=(f"x8_{g // 2}", "setup"), writes=(tpn,))
                if g % 2 == 0:
                    def fn(e, g=g, tpv=tpv, blk=blk):
                        ins = None
                        for j in range(8):
                            kc = g * 8 + j
                            ins = e.activation(out=hT3[:, kc, blk * 128:(blk + 1) * 128],
                                               in_=tpv[:, j * 128:(j + 1) * 128], func=AF.Identity,
                                               bias=modc[:, shcol + kc:shcol + kc + 1],
                                               scale=gs[:, gscol + kc:gscol + kc + 1])
                        return ins
                    P.op("act", fn, reads=(tpn, "modc", "gs"), writes=(f"hT{blk}_{g}",))
                else:
                    def fn(e, g=g, tpv=tpv, blk=blk):
                        ins = None
                        for j in range(8):
                            kc = g * 8 + j
                            ins = e.tensor_scalar(out=hT3[:, kc, blk * 128:(blk + 1) * 128],
                                                  in0=tpv[:, j * 128:(j + 1) * 128],
                                                  scalar1=gs[:, gscol + kc:gscol + kc + 1],
                                                  scalar2=modc[:, shcol + kc:shcol + kc + 1],
                                                  op0=ALU.mult, op1=ALU.add)
                        return ins
                    P.op("dve", fn, reads=(tpn, "modc", "gs"), writes=(f"hT{blk}_{g}",))

    def proj_chunk(nm, wv, j, ps_ap, pname):
        pairs = [(wv[:, kc, j * 128:(j + 1) * 128], hT3[:, kc, :]) for kc in range(KC)]
        P.op("pe", mm_group(ps_ap, pairs), reads=(nm,) + HT_ALL, writes=(pname,))

    def mixer_tile(l, tt):
        s_rot = Rot([("ps2", 2), ("ps3", 3)])
        pt_rot = Rot([("ps4", 4), ("ps5", 5)])
        cat_rot = Rot([("ps6", 6), ("ps7", 7)])
        ssb_rot = Rot([0, 1])
        pn_rot = Rot(list(range(16)))
        pts_rot = Rot(list(range(2)))
        ast_rot = Rot(list(range(16)))
        if tt == 0:
            def fn(e):
                e.memset(kT3[:, :, :], 0.0)
                e.memset(U[:, 21504:21504 + 3840], 0.0)
                return e.memset(utail[:, :], 0.0)
            P.op("dve", fn, reads=(), writes=KT_ALL + VP_ALL + ("utail",))
        else:
            def fn(e):
                e.tensor_copy(out=kT3[:, :, 0:128], in_=kT3[:, :, 512:640])
                return e.tensor_copy(out=vpad4[:, 0, :, :], in_=vpad4[:, 4, :, :])
            P.op("dve", fn, reads=("vpad4",), writes=KT_ALL + ("vpad0",))
        nm, wv = ws.get(kview(w_in_d[l, :, 2048:2304]), (KC, 256))
        for kh in range(4):
            pname, pi = mm_rot.next()
            ps = psf(pi)

            def fn(e, wv=wv, kh=kh, ps=ps):
                ins = None
                for half in range(2):
                    for kc in range(KC):
                        ins = e.matmul(ps[half * 64:(half + 1) * 64, :], wv[:, kc, kh * 64:(kh + 1) * 64],
                                       hT3[:, kc, :], start=(kc == 0), stop=(kc == KC - 1))
                return ins
            P.op("pe", fn, reads=(nm,) + HT_ALL, writes=(pname,))
            def fn(e, kh=kh, ps=ps):
                e.activation(out=kT3[0:64, 2 * kh, 128:640], in_=ps[0:64, :], func=AF.Copy)
                return e.activation(out=kT3[64:128, 2 * kh + 1, 128:640], in_=ps[64:128, :], func=AF.Copy)
            P.op("act", fn, reads=(pname,), writes=(f"kT{kh}",))
        nm, wv = ws.get(kview(w_in_d[l, :, 2304:2560]), (KC, 256))
        for blk in range(NB):
            pname, pi = mm_rot.next()
            ps = psf(pi)
            pairs = [(hT3[:, kc, blk * 128:(blk + 1) * 128], wv[:, kc, :]) for kc in range(KC)]
            P.op("pe", mm_group(ps[:, 0:256], pairs), reads=(nm,) + HT_ALL, writes=(pname,))
            P.op("dve", (lambda e, blk=blk, ps=ps: e.tensor_copy(
                out=vpad4[:, blk + 1, :, 64:128], in_=ps[:, 0:256].rearrange("p (h d) -> p h d", d=64))),
                reads=(pname,), writes=(f"vpad{blk + 1}",))
        parts = (stage or {}).get("parts", "kuqop")
        for g in range(4 if "u" in parts else 0):
            w = 2 ** (g + 1)
            for pr in range(2):
                cb = 2560 + g * 512 + pr * 256
                nm, wv = ws.get(kview(w_in_d[l, :, cb:cb + 256]), (KC, 256))
                for j in range(2):
                    ic = pr * 2 + j
                    c = g * 4 + ic
                    pname, pi = mm_rot.next()
                    ps = psf(pi)
                    proj_chunk(nm, wv, j, ps, pname)
                    P.op("dve", (lambda e, c=c: e.tensor_copy(out=ucur[:, 0:16], in_=utail[:, c * 16:(c + 1) * 16])),
                         reads=("utail",), writes=("ucur",))
                    P.op("act", (lambda e, ps=ps: e.activation(out=ucur[:, 16:528], in_=ps, func=AF.Copy)),
                         reads=(pname,), writes=("ucur",))

                    P.op("dve", (lambda e, c=c: e.tensor_copy(out=utail[:, c * 16:(c + 1) * 16], in_=ucur[:, 512:528])),
                         reads=("ucur",), writes=("utail",))
                    cur, curn = ucur, "ucur"
                    for lev in range(g + 1):
                        sh = 2 ** lev
                        lo = 2 * sh - 1
                        dst, dstn = stt_[lev % 2], f"st{lev % 2}"
                        P.op("dve", (lambda e, dst=dst, cur=cur, lo=lo, sh=sh: e.tensor_tensor(
                            out=dst[:, lo:528], in0=cur[:, lo:528], in1=cur[:, lo - sh:528 - sh], op=ALU.add)),
                            reads=(curn,), writes=(dstn,))
                        cur, curn = dst, dstn
                    P.op("dve", (lambda e, cur=cur, ic=ic, w=w: e.scalar_tensor_tensor(
                        out=pooled3[:, ic, :], in0=cur[:, 16:528], scalar=1.0 / w, in1=ucur[:, 16:528],
                        op0=ALU.mult, op1=ALU.subtract)), reads=(curn, "ucur"), writes=(f"pooled{ic}",))
                    if tt == 0:
                        P.op("dve", (lambda e, cur=cur, w=w: e.tensor_tensor(
                            out=fixb[:, 0:w - 1], in0=cur[:, 16:16 + w - 1], in1=cst[:, C_INV:C_INV + w - 1],
                            op=ALU.mult)), reads=(curn, "setup"), writes=("fixb",))
                        P.op("dve", (lambda e, ic=ic, w=w: e.tensor_tensor(
                            out=pooled3[:, ic, 0:w - 1], in0=fixb[:, 0:w - 1], in1=ucur[:, 16:16 + w - 1],
                            op=ALU.subtract)), reads=("fixb", "ucur"), writes=(f"pooled{ic}",))
            nm, wv = ws.get(kview(w_pool_d[l, g]), (4, 512))
            for oc in range(4):
                pname, pi = mm_rot.next()
                ps = psf(pi)
                pairs = [(wv[:, ic, oc * 128:(oc + 1) * 128], pooled3[:, ic, :]) for ic in range(4)]
                P.op("pe", mm_group(ps, pairs), reads=(nm,) + PO_ALL, writes=(pname,))
                col = l * 16 + g * 4 + oc
                P.op("act", (lambda e, ps=ps, col=col, cc=16 + g * 4 + oc: e.activation(
                    out=catT3[:, cc, :], in_=ps, func=AF.Identity, scale=pscale[:, col:col + 1])),
                    reads=(pname, "setup"), writes=(f"catT{16 + g * 4 + oc}",))

        def emit_pv(c, units):
            kh = c // 4
            cname, ci = cat_rot.next()
            cps = psf(ci)
            for bp in range(2):
                batch = []
                for blk in (2 * bp, 2 * bp + 1):
                    for hh in range(2):
                        batch.append((blk, hh, units[(hh, blk)]))
                ptn, pti = pt_rot.next()
                ptv = psh(pti)
                si = pts_rot.next()

                def fn(e, batch=batch, ptv=ptv):
                    ins = None
                    for k, (blk, hh, pni) in enumerate(batch):
                        for jc in range(2):
                            ins = e.transpose(out=ptv[:, k * 256 + jc * 128:k * 256 + (jc + 1) * 128],
                                              in_=pnb[pni][:, jc * 128:(jc + 1) * 128], identity=ident[:, :])
                    return ins
                P.op("pe", fn, reads=tuple(f"pn{pni}" for (_, _, pni) in batch) + ("setup",), writes=(ptn,))
                if bp == 0:
                    P.op("dve", (lambda e, si=si, ptv=ptv: e.tensor_copy(out=pts[si], in_=ptv)),
                         reads=(ptn,), writes=(f"pts{si}",))
                else:
                    P.op("act", (lambda e, si=si, ptv=ptv: e.activation(out=pts[si], in_=ptv, func=AF.Copy)),
                         reads=(ptn,), writes=(f"pts{si}",))

                def fn(e, batch=batch, si=si):
                    ins = None
                    for k, (blk, hh, pni) in enumerate(batch):
                        vs = slice(64, 192) if hh == 0 else slice(0, 128)
                        for jc in range(2):
                            ins = e.matmul(cps[:, blk * 128:(blk + 1) * 128], vpad4[:, blk + jc, kh, vs],
                                           pts[si][:, k * 256 + jc * 128:k * 256 + (jc + 1) * 128],
                                           start=(hh == 0 and jc == 0), stop=(hh == 1 and jc == 1))
                    return ins
                P.op("pe", fn, reads=(f"pts{si}",) + tuple(f"vpad{2 * bp + k}" for k in range(3)), writes=(cname,))
            P.op("act", (lambda e, c=c: e.activation(out=catT3[:, c, :], in_=cps, func=AF.Copy)),
                 reads=(cname,), writes=(f"catT{c}",))

        pending = None
        for qt in range(8 if "q" in parts else 0):
            nm, wv = ws.get(kview(w_in_d[l, :, qt * 256:(qt + 1) * 256]), (KC, 256))
            for j in range(2):
                c = 2 * qt + j
                kh = c // 4
                qb = c % 2
                pname, pi = mm_rot.next()
                ps = psf(pi)
                proj_chunk(nm, wv, j, ps, pname)
                P.op("act", (lambda e, qb=qb, ps=ps: e.activation(out=qT[qb], in_=ps, func=AF.Copy, scale=0.125)),
                     reads=(pname,), writes=(f"qT{qb}",))
                units = {}
                for blk in range(NB):
                    sname, si_ = s_rot.next()
                    spb = psf(si_)

                    def fn(e, qb=qb, blk=blk, kh=kh, spb=spb):
                        ins = None
                        for hh in range(2):
                            ins = e.matmul(spb[:, hh * 256:(hh + 1) * 256],
                                           qT[qb][:, blk * 128:(blk + 1) * 128],
                                           kT3[:, 2 * kh + hh, blk * 128:blk * 128 + 256],
                                           start=True, stop=True)
                        return ins
                    P.op("pe", fn, reads=(f"qT{qb}", f"kT{kh}"), writes=(sname,))
                    for hh in range(2):
                        h = 2 * c + hh
                        first = (tt == 0 and blk == 0)
                        sps = spb[:, hh * 256:(hh + 1) * 256]
                        sb_i = ssb_rot.next()
                        a0 = ast_rot.next() * 8
                        pni = pn_rot.next()
                        dmc = C_DM0 if first else C_DM
                        sk = l * 32 + h

                        P.op("dve", (lambda e, sb_i=sb_i, h=h, sps=sps, dmc=dmc: e.scalar_tensor_tensor(
                            out=ssb[sb_i], in0=cst[:, dmc:dmc + 256], scalar=cst[:, C_NSL + h:C_NSL + h + 1],
                            in1=sps, op0=ALU.mult, op1=ALU.add)), reads=(sname, "setup"), writes=(f"ssb{sb_i}",))
                        P.op("dve", (lambda e, sb_i=sb_i, a0=a0: e.reduce_max(
                            out=ast[:, a0:a0 + 1], in_=ssb[sb_i], axis=AX.X)),
                            reads=(f"ssb{sb_i}",), writes=(f"ast{a0}",))

                        def fn(e, a0=a0, sk=sk):
                            e.memset(ast[:, a0 + 2:a0 + 3], 0.0)
                            return e.tensor_scalar(out=ast[:, a0 + 1:a0 + 2], in0=ast[:, a0:a0 + 1],
                                                   scalar1=sinks[:, sk:sk + 1], scalar2=-1.0,
                                                   op0=ALU.max, op1=ALU.mult)
                        P.op("dve", fn, reads=(f"ast{a0}", "setup"), writes=(f"ast{a0}",))

                        def fn(e, sb_i=sb_i, a0=a0, sk=sk):
                            e.activation(out=pexp[sb_i], in_=ssb[sb_i], func=AF.Exp, bias=ast[:, a0 + 1:a0 + 2],
                                         scale=1.0, accum_out=ast[:, a0 + 2:a0 + 3])
                            return e.activation(out=ast[:, a0 + 3:a0 + 4], in_=sinks[:, sk:sk + 1], func=AF.Exp,
                                                bias=ast[:, a0 + 1:a0 + 2], scale=1.0)
                        P.op("act", fn, reads=(f"ssb{sb_i}", f"ast{a0}", "setup"), writes=(f"pexp{sb_i}", f"ast{a0}"))

                        P.op("dve", (lambda e, a0=a0: e.tensor_tensor(
                            out=ast[:, a0 + 4:a0 + 5], in0=ast[:, a0 + 2:a0 + 3], in1=ast[:, a0 + 3:a0 + 4],
                            op=ALU.add)), reads=(f"ast{a0}",), writes=(f"ast{a0}",))
                        P.op("dve", (lambda e, a0=a0: e.reciprocal(out=ast[:, a0 + 5:a0 + 6],
                                                                   in_=ast[:, a0 + 4:a0 + 5])),
                             reads=(f"ast{a0}",), writes=(f"ast{a0}",))
                        P.op("act", (lambda e, sb_i=sb_i, a0=a0, pni=pni: e.activation(
                            out=pnb[pni], in_=pexp[sb_i], func=AF.Identity, scale=ast[:, a0 + 5:a0 + 6])),
                            reads=(f"pexp{sb_i}", f"ast{a0}"), writes=(f"pn{pni}",))
                        units[(hh, blk)] = pni
                if pending is not None:
                    emit_pv(*pending)
                pending = (c, units)
        if pending is not None:
            emit_pv(*pending)
        if "o" not in parts:
            return

        P.op("dve", lambda e: e.memset(ssp[:, :], 0.0), reads=(), writes=tuple(f"ssp{b}" for b in range(NB)))
        o_rot = Rot([("ps0", 0), ("ps1", 1), ("ps2", 2), ("ps3", 3)])
        for n in range(16):
            nm, wv = ws.get(kview(w_out_d[l, :, n * 256:(n + 1) * 256]), (KC, 256))
            for blk in range(NB):
                pname, pi = o_rot.next()
                ps = psf(pi)[:, 0:256]
                pairs = [(catT3[:, c, blk * 128:(blk + 1) * 128], wv[:, c, :]) for c in range(KC)]
                P.op("pe", mm_group(ps, pairs), reads=(nm,) + CAT_ALL, writes=(pname,))
                P.op("act", (lambda e, ps=ps, blk=blk, n=n: e.activation(
                    out=junk[:, blk * 256:(blk + 1) * 256], in_=ps, func=AF.Square,
                    accum_out=ssp[:, blk * 16 + n:blk * 16 + n + 1])),
                    reads=(pname,), writes=(f"junk{blk}", f"ssp{blk}"))
                P.op("dve", (lambda e, ps=ps, blk=blk, n=n: e.tensor_copy(
                    out=mixb3[:, blk, n * 256:(n + 1) * 256], in_=ps)), reads=(pname,), writes=(f"mixb{blk}",))

    def post(l, sub, tt, xsrc, xname, xdst, dname):
        gi = l * 2 + sub
        for blk in range(NB):
            gb = tt * NB + blk
            r0 = gb * 128
            sc = 16 + (gb % 2) * 8
            stn = f"pstat{gb % 2}"
            if sub == 0:
                P.op("dve", (lambda e, blk=blk, sc=sc: e.reduce_sum(out=stat[:, sc + 2:sc + 3],
                                                                    in_=ssp[:, blk * 16:(blk + 1) * 16], axis=AX.X)),
                     reads=(f"ssp{blk}",), writes=(stn,))
            else:
                P.op("dve", (lambda e, sc=sc: e.memset(stat[:, sc:sc + 2], 0.0)), reads=(), writes=(stn,))
                for h in range(2):
                    P.op("act", (lambda e, h=h, blk=blk, sc=sc: e.activation(
                        out=xn[:, h * 2048:(h + 1) * 2048],
                        in_=acc3[:, blk, h * 2048:(h + 1) * 2048], func=AF.Square,
                        accum_out=stat[:, sc + h:sc + h + 1])), reads=(f"acc{blk}",), writes=(f"x8_{h}", stn))
                P.op("dve", (lambda e, sc=sc: e.tensor_tensor(out=stat[:, sc + 2:sc + 3], in0=stat[:, sc:sc + 1],
                                                              in1=stat[:, sc + 1:sc + 2], op=ALU.add)),
                     reads=(stn,), writes=(stn,))
            P.op("act", (lambda e, sc=sc: e.activation(out=stat[:, sc + 3:sc + 4], in_=stat[:, sc + 2:sc + 3],
                                                       func=AF.Sqrt, bias=cst[:, C_EPS:C_EPS + 1], scale=1.0 / D)),
                 reads=(stn, "setup"), writes=(stn,))
            P.op("dve", (lambda e, sc=sc: e.reciprocal(out=stat[:, sc + 4:sc + 5], in_=stat[:, sc + 3:sc + 4])),
                 reads=(stn,), writes=(stn,))
            for h in range(2):
                P.op("sp", (lambda e, h=h, r0=r0: e.dma_start(out=xin[h][:, :],
                                                               in_=xsrc[r0:r0 + 128, h * 2048:(h + 1) * 2048])),
                     reads=(f"{xname}:{gb}:{h}",), writes=(f"xin{h}",), dma=f"xin{h}")
                P.op("sp", (lambda e, h=h: e.dma_start(
                    out=gph, in_=gps_d[gi:gi + 1, h * 2048:(h + 1) * 2048].to_broadcast([128, 2048]))),
                    reads=("gps",), writes=("x8_0", "x8_1"), dma="x8")
                src_ = (mixb3 if sub == 0 else acc3)
                srcn = f"mixb{blk}" if sub == 0 else f"acc{blk}"

                P.op("dve", (lambda e, h=h, blk=blk, sc=sc, src_=src_: e.scalar_tensor_tensor(
                    out=gph, in0=src_[:, blk, h * 2048:(h + 1) * 2048], scalar=stat[:, sc + 4:sc + 5], in1=gph,
                    op0=ALU.mult, op1=ALU.mult)), reads=(srcn, stn, "x8_0", "x8_1"), writes=("x8_0", "x8_1"))
                P.op("dve", (lambda e, h=h: e.tensor_tensor(out=xin[h][:, :], in0=xin[h][:, :], in1=gph, op=ALU.add)),
                     reads=("x8_0", "x8_1", f"xin{h}"), writes=(f"xin{h}",))
                P.op("sp", (lambda e, h=h, r0=r0: e.dma_start(out=xdst[r0:r0 + 128, h * 2048:(h + 1) * 2048],
                                                               in_=xin[h][:, :])),
                     reads=(f"xin{h}",), writes=(f"{dname}:{gb}:{h}",), dma=f"xin{h}")

    def ffn_tile(l, tt):
        moe = (l % 2 == 1) or bool((stage or {}).get("force_moe"))
        dn_rot = Rot([("ps4", 4), ("ps5", 5)])
        if moe:
            for blk in range(NB):
                ps = psf(6)[:, 0:NE]
                pairs = [(hT3[:, kc, blk * 128:(blk + 1) * 128], rw3[:, kc, :]) for kc in range(KC)]
                P.op("pe", mm_group(ps, pairs), reads=HT_ALL + ("rw",), writes=("ps6",))
                r0 = blk * 16

                lg = rstat[:, r0:r0 + 8]
                mx = rstat[:, r0 + 8:r0 + 16]
                cb = comb[:, blk * 8:(blk + 1) * 8]
                P.op("dve", (lambda e, ps=ps, lg=lg: e.tensor_tensor(out=lg, in0=ps, in1=rb[:, :], op=ALU.add)),
                     reads=("ps6", "setup"), writes=("rstat",))
                P.op("dve", (lambda e, lg=lg, mx=mx: e.max(out=mx, in_=lg)), reads=("rstat",), writes=("rstat",))

                def fn(e, lg=lg, mx=mx, cb=cb, r0=r0):
                    e.tensor_scalar(out=cb, in0=lg, scalar1=mx[:, 1:2], scalar2=None, op0=ALU.is_ge)
                    return e.tensor_scalar(out=rstat[:, r0 + 16 - 6:r0 + 16 - 5], in0=mx[:, 0:1], scalar1=-1.0,
                                           scalar2=None, op0=ALU.mult)
                P.op("dve", fn, reads=("rstat",), writes=("rstat", "comb"))
                P.op("act", (lambda e, lg=lg, r0=r0: e.activation(out=lg, in_=lg, func=AF.Exp,
                                                                  bias=rstat[:, r0 + 10:r0 + 11], scale=1.0)),
                     reads=("rstat",), writes=("rstat",))
                P.op("dve", (lambda e, cb=cb, lg=lg: e.tensor_tensor(out=cb, in0=cb, in1=lg, op=ALU.mult)),
                     reads=("rstat", "comb"), writes=("comb",))
                P.op("dve", (lambda e, cb=cb, r0=r0: e.reduce_sum(out=rstat[:, r0 + 11:r0 + 12], in_=cb, axis=AX.X)),
                     reads=("comb",), writes=("rstat",))
                P.op("dve", (lambda e, r0=r0: e.reciprocal(out=rstat[:, r0 + 12:r0 + 13],
                                                           in_=rstat[:, r0 + 11:r0 + 12])),
                     reads=("rstat",), writes=("rstat",))
                P.op("dve", (lambda e, cb=cb, r0=r0: e.tensor_scalar(out=cb, in0=cb, scalar1=rstat[:, r0 + 12:r0 + 13],
                                                                     scalar2=None, op0=ALU.mult)),
                     reads=("rstat", "comb"), writes=("comb",))
            groups = []
            for ex in range(NE):
                for hg in range(2):
                    groups.append((mg_d[0, ex], mu_d[0, ex], md_d[0, ex], hg * 16, 16, ex))
        else:
            groups = []
            for g0 in range(0, D_FF // 128, 16):
                groups.append((fg_d[0], fu_d[0], fd_d[0], g0, min(16, D_FF // 128 - g0), None))
        first = True
        for (Wg, Wu, Wd, g0, nch, ex) in groups:
            for p in range(nch // 2):
                c0 = (g0 + 2 * p) * 128
                nmg, wg = ws.get(kview(Wg[:, c0:c0 + 256]), (KC, 256))
                for j in range(2):
                    proj_chunk(nmg, wg, j, psf(j), f"ps{j}")
                    P.op("act", (lambda e, j=j: e.activation(out=sg[j], in_=psf(j), func=AF.Silu)),
                         reads=(f"ps{j}",), writes=(f"sg{j}",))
                nmu, wu = ws.get(kview(Wu[:, c0:c0 + 256]), (KC, 256))
                for j in range(2):
                    proj_chunk(nmu, wu, j, psf(2 + j), f"ps{2 + j}")
                    P.op("dve", (lambda e, j=j, p=p: e.tensor_tensor(out=hid3[:, 2 * p + j, :], in0=sg[j],
                                                                     in1=psf(2 + j), op=ALU.mult)),
                         reads=(f"sg{j}", f"ps{2 + j}"), writes=(f"hid{2 * p + j}",))
            for n in range(8):
                nmd, wd = ws.get(kview(Wd[g0 * 128:(g0 + nch) * 128, n * 512:(n + 1) * 512]), (nch, 512))
                for blk in range(NB):
                    pname, pi = dn_rot.next()
                    ps = psf(pi)
                    pairs = [(hid3[:, j, blk * 128:(blk + 1) * 128], wd[:, j, :]) for j in range(nch)]
                    P.op("pe", mm_group(ps, pairs), reads=(nmd,) + HID_ALL[:nch], writes=(pname,))
                    accv = acc3[:, blk, n * 512:(n + 1) * 512]
                    if ex is None:
                        if first:
                            P.op("dve", (lambda e, ps=ps, accv=accv: e.tensor_copy(out=accv, in_=ps)),
                                 reads=(pname,), writes=(f"acc{blk}",))
                        else:
                            P.op("dve", (lambda e, ps=ps, accv=accv: e.tensor_tensor(out=accv, in0=ps, in1=accv,
                                                                                     op=ALU.add)),
                                 reads=(pname, f"acc{blk}"), writes=(f"acc{blk}",))
                    else:
                        cbv = comb[:, blk * 8 + ex:blk * 8 + ex + 1]
                        if first:
                            P.op("dve", (lambda e, ps=ps, accv=accv, cbv=cbv: e.tensor_scalar(
                                out=accv, in0=ps, scalar1=cbv, scalar2=None, op0=ALU.mult)),
                                reads=(pname, "comb"), writes=(f"acc{blk}",))
                        else:
                            P.op("dve", (lambda e, ps=ps, accv=accv, cbv=cbv: e.scalar_tensor_tensor(
                                out=accv, in0=ps, scalar=cbv, in1=accv, op0=ALU.mult, op1=ALU.add)),
                                reads=(pname, "comb", f"acc{blk}"), writes=(f"acc{blk}",))
            first = False

    def emit_all():
        nonlocal tp_rot, mm_rot
        tp_rot = Rot([("ps6", 6), ("ps7", 7)])
        mm_rot = Rot([("ps0", 0), ("ps1", 1)])
        emit_setup()
        st = stage or {}
        nl = st.get("layers", 2)
        for l in range(nl):
            src, sname = (x_d, "x") if l == 0 else (xs2_d, "xs2")
            for tt in range(st.get("mix_tiles", NT)):
                phase_A(tt, src, sname, l, 0)
                if st.get("only_A"):
                    continue
                mixer_tile(l, tt)
                if "p" in st.get("parts", "kuqop"):
                    post(l, 0, tt, src, sname, xs1_d, "xs1")
            dst, dname = (xs2_d, "xs2") if l == 0 else (out_d, "out")
            nft = st.get("ffn_tiles", NT) if l == nl - 1 else NT
            for tt in range(nft):
                phase_A(tt, xs1_d, "xs1", l, 1)
                ffn_tile(l, tt)
                post(l, 1, tt, xs1_d, "xs1", dst, dname)
        if dbgmode and not P.dry:
            dl = [(dbg_d[:, 0:384], modc[:, :]), (dbg_d[:, 384:512], gs[:, :]), (dbg_d[:, 512:640], gpc[:, :]),
                  (dbg_d[:, 1024:1024 + 2048], hT[:, 0:4096].bitcast(F32))]
            for o_, i_ in dl:
                P.op("sp", (lambda e, o_=o_, i_=i_: e.dma_start(out=o_, in_=i_)),
                     reads=("modc", "gs", "gpc") + HT_ALL, writes=("dbgout",), dma="dbgo")
        names = [n for n in P.bufs if n.split(":")[0] in ("out", "xs1", "xs2", "dbgout")]
        P.op("sp", lambda e: e.dma_start(out=fin_d[0:1, 0:64], in_=cst[0:1, 0:64]), reads=tuple(names) + ("setup",),
             writes=("fin",), dma="fin")

    P.dry = True
    emit_all()
    plan = ws.plan
    P.reset()
    P.overlaps = overlaps0
    ws.plan = plan
    ws.pos = 0
    ws.loaded = 0
    emit_all()
    assert ws.pos == len(plan), (ws.pos, len(plan))

    with ExitStack() as es:
        sems = {}
        for key in P.cnt:
            sems[key] = es.enter_context(nc.semaphore(key.replace(":", "_")))
        fin_tok = P.cnt["d:fin"]
        block = es.enter_context(nc.Block())

        def replay(name, eng, tail=None):
            for waits, fn, key, inc in P.streams[name]:
                for k, v in waits:
                    eng.wait_ge(sems[k], v)
                ins = fn(eng)
                ins.then_inc(sems[key], inc)
            if tail:
                tail(eng)

        @block.sync
        def _(e):
            replay("sp", e, tail=lambda e: e.wait_ge(sems["d:fin"], fin_tok))

        @block.gpsimd
        def _(e):
            replay("pool", e)

        @block.tensor
        def _(e):
            replay("pe", e)

        @block.scalar
        def _(e):
            replay("act", e)

        @block.vector
        def _(e):
            replay("dve", e)
    return nc


_NC = None


def _consts():
    i = np.arange(128)[:, None]
    j = np.arange(256)[None, :]
    d = (i + 128 - j).astype(np.float32)
    valid = (d >= 0) & (d < 128)
    dm = np.where(valid, d, BIG).astype(np.float32)
    dm0 = np.where(valid & (j >= 128), d, BIG).astype(np.float32)
    h = np.arange(1, 33, dtype=np.float32)
    nsl = -np.exp2(-8.0 * h / 32.0).astype(np.float32)
    cst = np.zeros((128, CW), np.float32)
    cst[:, C_DM:C_DM + 256] = dm
    cst[:, C_DM0:C_DM0 + 256] = dm0
    cst[:, C_NSL:C_NSL + 32] = nsl[None, :]
    cst[:, C_INV:C_INV + 16] = (1.0 / np.arange(1, 17, dtype=np.float32))[None, :]
    cst[:, C_EPS] = EPS
    return cst


def kernel(x, c, w_ada, b_ada, norm_pre_mix, norm_post_mix, norm_pre_ffn, norm_post_ffn,
           w_in, sinks, w_pool, pool_scale, w_out, ffn_w_gate, ffn_w_up, ffn_w_down,
           router_w, router_b, moe_w_gate, moe_w_up, moe_w_down):
    global _NC
    if _NC is None:
        _NC = build_program()
    nc = _NC
    in_maps = prep_inputs(x, c, w_ada, b_ada, norm_pre_mix, norm_post_mix, norm_pre_ffn, norm_post_ffn,
                          w_in, sinks, w_pool, pool_scale, w_out, ffn_w_gate, ffn_w_up, ffn_w_down,
                          router_w, router_b, moe_w_gate, moe_w_up, moe_w_down)
    res = run_bass_kernel_spmd(nc, in_maps, core_ids=list(range(NCORES)))
    out = np.stack([np.asarray(res.results[b]["out"], dtype=np.float32) for b in range(NCORES)], 0)
    return out


def prep_inputs(x, c, w_ada, b_ada, norm_pre_mix, norm_post_mix, norm_pre_ffn, norm_post_ffn,
                w_in, sinks, w_pool, pool_scale, w_out, ffn_w_gate, ffn_w_up, ffn_w_down,
                router_w, router_b, moe_w_gate, moe_w_up, moe_w_down):
    f = lambda a: np.ascontiguousarray(np.asarray(a, dtype=np.float32))
    x = f(x); c = f(c)
    b_adaT = np.ascontiguousarray(f(b_ada).reshape(2, 192, 128).transpose(2, 0, 1).reshape(128, 384))
    gains = np.stack([f(norm_pre_mix), f(norm_post_mix), f(norm_pre_ffn), f(norm_post_ffn)], 0)
    gainsT = np.ascontiguousarray(gains.reshape(4, 2, 32, 128).transpose(3, 0, 1, 2).reshape(128, 256))
    sinks_b = np.ascontiguousarray(np.broadcast_to(f(sinks).reshape(1, 64), (128, 64)))
    pscaleT = np.ascontiguousarray(f(pool_scale).reshape(2, 16, 128).transpose(2, 0, 1).reshape(128, 32))
    rb_b = np.ascontiguousarray(np.broadcast_to(f(router_b).reshape(1, NE), (128, NE)))
    ident = np.eye(128, dtype=np.float32).astype(ml_dtypes.bfloat16)
    cst = _consts()
    shared = {
        "w_ada": f(w_ada), "b_adaT": b_adaT, "gainsT": gainsT, "w_in": f(w_in), "sinks_b": sinks_b,
        "w_pool": f(w_pool), "pool_scaleT": pscaleT, "w_out": f(w_out), "ffn_w_gate": f(ffn_w_gate),
        "ffn_w_up": f(ffn_w_up), "ffn_w_down": f(ffn_w_down), "router_w": f(router_w), "router_b_b": rb_b,
        "moe_w_gate": f(moe_w_gate), "moe_w_up": f(moe_w_up), "moe_w_down": f(moe_w_down),
        "ident": ident, "cst": cst,
    }
    in_maps = []
    for b in range(NCORES):
        m = dict(shared)
        m["x"] = x[b]
        m["cT"] = np.ascontiguousarray(c[b].reshape(32, 128).T)
        in_maps.append(m)
    return in_maps
```
